# Optimizing a Trainium2 kernel written in Bass

```python
import math
import jax, jax.numpy as jnp
from jax import lax
import numpy as np

D_MODEL = 2048
BATCH = 1
SEQ = 8192
DEPTH = 4

GRID_W = 64
CTX_LEN = 256
HEAD_DIM = 128
BRANCH_WIDTH = 1024
A_Q_HEADS = 8
A_KV_HEADS = 2
WINDOW = 128
A_BLOCK = 128
B_GROUPS = 8
B_GROUP_DIM = 128
B_CHUNK = 128
C_HEADS = 8
C_CHUNK = 128
N_GROUPS = 4
EXPERTS_PER_GROUP = 8
TOP_K = 2
D_EXPERT = 512
MOE_BLOCK = 128

N_BRANCH = 3
ROPE_BASE = 10000.0
EPS = 1e-6
NEG_INF = -1e30
F32 = jnp.float32

A_WIDTH = A_Q_HEADS * HEAD_DIM
A_KV_WIDTH = A_KV_HEADS * HEAD_DIM
B_WIDTH = B_GROUPS * B_GROUP_DIM
C_WIDTH = C_HEADS * HEAD_DIM
N_EXPERTS = N_GROUPS * EXPERTS_PER_GROUP
IN_SIZES = (A_KV_WIDTH, A_KV_WIDTH, C_WIDTH, C_WIDTH, A_WIDTH, C_WIDTH, C_WIDTH, B_WIDTH, B_WIDTH, N_BRANCH * D_MODEL)
IN_WIDTH = sum(IN_SIZES)
IN_SPLITS = tuple(int(s) for s in np.cumsum(IN_SIZES)[:-1])
CTX_STATE_WIDTH = 2 * A_KV_WIDTH + 2 * C_WIDTH

kernel_name = "hybrid_dit_window_gmlp_retention_hmoe"


def rmsnorm(x, g):
    xf = x.astype(F32)
    y = xf * lax.rsqrt(jnp.mean(xf * xf, axis=-1, keepdims=True) + EPS)
    return (y * g.astype(F32)).astype(x.dtype)


def groupnorm(x, g):
    xf = x.astype(F32)
    xc = xf - jnp.mean(xf, axis=-1, keepdims=True)
    y = xc * lax.rsqrt(jnp.mean(xc * xc, axis=-1, keepdims=True) + EPS)
    return (y * g.astype(F32)).astype(x.dtype)


def modulate(h, shift, scale):
    return h * (1.0 + scale) + shift


def axial_rope_tables(n):
    rows = n // GRID_W
    assert rows * GRID_W == n
    row = jnp.repeat(jnp.arange(rows, dtype=F32), GRID_W)
    col = jnp.tile(jnp.arange(GRID_W, dtype=F32), rows)
    nq = HEAD_DIM // 4
    inv = ROPE_BASE ** (-jnp.arange(nq, dtype=F32) / nq)
    ang = jnp.stack([row[:, None] * inv, col[:, None] * inv], axis=1)
    return jnp.cos(ang), jnp.sin(ang)


def apply_rope(x, cos, sin):
    xf = x.astype(F32)
    x4 = xf.reshape(*x.shape[:-1], 2, 2, HEAD_DIM // 4)
    x1, x2 = x4[..., 0, :], x4[..., 1, :]
    cs, sn = cos[:, None], sin[:, None]
    out = jnp.stack([x1 * cs - x2 * sn, x2 * cs + x1 * sn], axis=-2)
    return out.reshape(x.shape).astype(x.dtype)


def latent_window_attention(q, k, v, k_ctx, v_ctx, sink):
    B, N = q.shape[:2]
    G = A_Q_HEADS // A_KV_HEADS
    nb = N // A_BLOCK
    scale = HEAD_DIM ** -0.5
    qb = q.reshape(B, nb, A_BLOCK, A_KV_HEADS, G, HEAD_DIM)

    def band(t):
        pad = jnp.zeros((B, A_BLOCK) + t.shape[2:], t.dtype)
        tp = jnp.concatenate([pad, t, pad], axis=1).reshape(B, nb + 2, A_BLOCK, *t.shape[2:])
        return jnp.concatenate([tp[:, :-2], tp[:, 1:-1], tp[:, 2:]], axis=2)

    kb, vb = band(k), band(v)
    s_loc = jnp.einsum('bnqhgd,bnkhd->bhgnqk', qb, kb, preferred_element_type=F32) * scale
    q_pos = (jnp.arange(nb)[:, None] * A_BLOCK + jnp.arange(A_BLOCK)[None, :])[:, :, None]
    k_pos = ((jnp.arange(nb)[:, None] - 1) * A_BLOCK + jnp.arange(3 * A_BLOCK)[None, :])[:, None, :]
    valid = (jnp.abs(q_pos - k_pos) <= WINDOW) & (k_pos >= 0) & (k_pos < N)
    s_loc = jnp.where(valid, s_loc, NEG_INF)
    s_ctx = jnp.einsum('bnqhgd,blhd->bhgnql', qb, k_ctx, preferred_element_type=F32) * scale
    s_sink = jnp.broadcast_to(sink.astype(F32).reshape(1, A_KV_HEADS, G, 1, 1, 1), s_loc.shape[:-1] + (1,))
    p = jax.nn.softmax(jnp.concatenate([s_loc, s_ctx, s_sink], axis=-1), axis=-1).astype(v.dtype)
    nk = 3 * A_BLOCK
    o = (jnp.einsum('bhgnqk,bnkhd->bnqhgd', p[..., :nk], vb)
         + jnp.einsum('bhgnql,blhd->bnqhgd', p[..., nk:-1], v_ctx))
    return o.reshape(B, N, A_WIDTH)


def context_attention(q, k, v, sink):
    B, L = q.shape[:2]
    G = A_Q_HEADS // A_KV_HEADS
    qc = q.reshape(B, L, A_KV_HEADS, G, HEAD_DIM)
    s = jnp.einsum('blhgd,bmhd->bhglm', qc, k, preferred_element_type=F32) * (HEAD_DIM ** -0.5)
    s_sink = jnp.broadcast_to(sink.astype(F32).reshape(1, A_KV_HEADS, G, 1, 1), s.shape[:-1] + (1,))
    p = jax.nn.softmax(jnp.concatenate([s, s_sink], axis=-1), axis=-1).astype(v.dtype)
    o = jnp.einsum('bhglm,bmhd->blhgd', p[..., :-1], v)
    return o.reshape(B, L, A_WIDTH)


def chunk_gmlp(u, v, norm_g, w_s, b_s):
    B, n, _ = u.shape
    nc = n // B_CHUNK
    vh = groupnorm(v.reshape(B, nc, B_CHUNK, B_GROUPS, B_GROUP_DIM), norm_g)
    mixed = jnp.einsum('gpq,bcqgd->bcpgd', w_s, vh) + b_s.T[None, None, :, :, None]
    return u * mixed.reshape(B, n, B_WIDTH)


def retention_scan(k, v, log_gamma, state0, q=None):
    B, n, H, d = k.shape
    nc = n // C_CHUNK
    to_chunks = lambda t: t.astype(F32).reshape(B, nc, C_CHUNK, H, d).transpose(1, 0, 3, 2, 4)
    idx = jnp.arange(C_CHUNK, dtype=F32)
    lg = log_gamma.astype(F32)[:, None]
    diff = idx[:, None] - idx[None, :]
    intra = jnp.where(diff >= 0, jnp.exp(lg[:, :, None] * jnp.maximum(diff, 0.0)), 0.0)
    q_decay = jnp.exp(lg * (idx + 1.0))[:, :, None]
    k_decay = jnp.exp(lg * (C_CHUNK - 1.0 - idx))[:, :, None]
    chunk_decay = jnp.exp(lg[:, 0] * C_CHUNK)[:, None, None]

    def step(S, chunk):
        kc, vc = chunk[0], chunk[1]
        S_new = S * chunk_decay + jnp.einsum('bhjd,bhje->bhde', kc * k_decay, vc)
        if q is None:
            return S_new, None
        qc = chunk[2]
        s = jnp.einsum('bhid,bhjd->bhij', qc, kc) * intra
        out = jnp.einsum('bhij,bhje->bhie', s, vc) + jnp.einsum('bhid,bhde->bhie', qc * q_decay, S)
        return S_new, out

    if q is None:
        S_fin, _ = lax.scan(step, state0, (to_chunks(k), to_chunks(v)))
        return None, S_fin
    S_fin, out = lax.scan(step, state0, (to_chunks(k), to_chunks(v), to_chunks(q)))
    return out.transpose(1, 0, 3, 2, 4).reshape(B, n, H, d), S_fin


def retention_output(o, g, norm_g):
    y = groupnorm(o, norm_g).reshape(g.shape).astype(g.dtype)
    return jax.nn.silu(g) * y


def merge_branches(attn, gmlp, ret, gate_logits, w_branch, w_out):
    br = jnp.stack([attn, gmlp, ret], axis=2)
    proj = jnp.einsum('bnkw,kwd->bnkd', br, w_branch)
    g = jax.nn.sigmoid(gate_logits.reshape(*gate_logits.shape[:2], N_BRANCH, D_MODEL))
    return jnp.sum(g * proj, axis=2) @ w_out


def mixing_sublayer(hc, hx, with_ctx, cos, sin, w_in, a_q_norm, a_k_norm, a_sink, b_norm, b_spatial,
                    b_spatial_bias, c_decay_fwd, c_decay_bwd, c_norm, w_branch, w_out):
    B = hx.shape[0]
    heads = lambda t, h: t.reshape(t.shape[0], t.shape[1], h, HEAD_DIM)
    rope = lambda t: apply_rope(t, cos, sin)
    flip = lambda t: t[:, ::-1]
    lg_f = jax.nn.log_sigmoid(c_decay_fwd.astype(F32))
    lg_b = jax.nn.log_sigmoid(c_decay_bwd.astype(F32))

    (xa_k, xa_v, xr_k, xr_v, xa_q, xr_q, xr_g, xb_u, xb_v, x_gate) = jnp.split(hx @ w_in, IN_SPLITS, axis=-1)
    if with_ctx:
        (ca_k, ca_v, cr_k, cr_v, ca_q, cr_q, cr_g, cb_u, cb_v, c_gate) = jnp.split(hc @ w_in, IN_SPLITS, axis=-1)
    else:
        ca_k, ca_v, cr_k, cr_v = jnp.split(hc @ w_in[:, :CTX_STATE_WIDTH], IN_SPLITS[:3], axis=-1)

    ka_c = rmsnorm(heads(ca_k, A_KV_HEADS), a_k_norm)
    va_c = heads(ca_v, A_KV_HEADS)
    attn_x = latent_window_attention(rope(rmsnorm(heads(xa_q, A_Q_HEADS), a_q_norm)),
                                     rope(rmsnorm(heads(xa_k, A_KV_HEADS), a_k_norm)),
                                     heads(xa_v, A_KV_HEADS), ka_c, va_c, a_sink)
    gmlp_x = chunk_gmlp(jax.nn.gelu(xb_u), jax.nn.gelu(xb_v), b_norm, b_spatial, b_spatial_bias)
    k_scale = HEAD_DIM ** -0.5
    kr_c = heads(cr_k, C_HEADS) * k_scale
    vr_c = heads(cr_v, C_HEADS)
    s0 = jnp.zeros((B, C_HEADS, HEAD_DIM, HEAD_DIM), F32)
    if with_ctx:
        qr_c = heads(cr_q, C_HEADS)
        oc_f, s_f = retention_scan(kr_c, vr_c, lg_f, s0, qr_c)
        oc_b, s_b = retention_scan(flip(kr_c), flip(vr_c), lg_b, s0, flip(qr_c))
    else:
        _, s_f = retention_scan(kr_c, vr_c, lg_f, s0)
        _, s_b = retention_scan(flip(kr_c), flip(vr_c), lg_b, s0)
    qr_x = rope(heads(xr_q, C_HEADS))
    kr_x = rope(heads(xr_k, C_HEADS)) * k_scale
    vr_x = heads(xr_v, C_HEADS)
    ox_f, _ = retention_scan(kr_x, vr_x, lg_f, s_f, qr_x)
    ox_b, _ = retention_scan(flip(kr_x), flip(vr_x), lg_b, s_b, flip(qr_x))
    ret_x = retention_output(ox_f + flip(ox_b), xr_g, c_norm)

    out_x = merge_branches(attn_x, gmlp_x, ret_x, x_gate, w_branch, w_out)
    if not with_ctx:
        return None, out_x
    attn_c = context_attention(rmsnorm(heads(ca_q, A_Q_HEADS), a_q_norm), ka_c, va_c, a_sink)
    gmlp_c = chunk_gmlp(jax.nn.gelu(cb_u), jax.nn.gelu(cb_v), b_norm, b_spatial, b_spatial_bias)
    ret_c = retention_output(oc_f + flip(oc_b), cr_g, c_norm)
    out_c = merge_branches(attn_c, gmlp_c, ret_c, c_gate, w_branch, w_out)
    return out_c, out_x


def moe_ffn(h, w_rg, b_rg, w_re, b_re, w_e1, w_e2):
    T, D = h.shape
    hf = h.astype(F32)
    g_logits = hf @ w_rg.astype(F32) + b_rg.astype(F32)
    g_prob = jax.nn.softmax(g_logits, axis=-1)
    g_sel = jnp.argmax(g_logits, axis=-1).astype(jnp.int32)
    g_w = jnp.take_along_axis(g_prob, g_sel[:, None], axis=-1)
    e_logits = (hf @ w_re.astype(F32) + b_re.astype(F32)).reshape(T, N_GROUPS, EXPERTS_PER_GROUP)
    e_logits = jnp.take_along_axis(e_logits, g_sel[:, None, None], axis=1)[:, 0]
    top_p, top_i = lax.top_k(jax.nn.softmax(e_logits, axis=-1), TOP_K)
    top_p = top_p / jnp.sum(top_p, axis=-1, keepdims=True)
    weights = (g_w * top_p).reshape(-1)
    experts = (g_sel[:, None] * EXPERTS_PER_GROUP + top_i.astype(jnp.int32)).reshape(-1)
    tokens = jnp.repeat(jnp.arange(T, dtype=jnp.int32), TOP_K)

    A = T * TOP_K
    n_blocks = -(-(A + N_EXPERTS * (MOE_BLOCK - 1)) // MOE_BLOCK)
    P = n_blocks * MOE_BLOCK
    counts = jnp.zeros((N_EXPERTS,), jnp.int32).at[experts].add(1)
    padded = (counts + MOE_BLOCK - 1) // MOE_BLOCK * MOE_BLOCK
    pad_end = jnp.cumsum(padded)
    pad_start = pad_end - padded
    start = jnp.cumsum(counts) - counts
    order = jnp.argsort(experts)
    e_sorted = experts[order]
    dest = pad_start[e_sorted] + jnp.arange(A, dtype=jnp.int32) - start[e_sorted]
    slot_tok = jnp.zeros((P,), jnp.int32).at[dest].set(tokens[order])
    slot_w = jnp.zeros((P,), F32).at[dest].set(weights[order]).astype(h.dtype)
    block_e = jnp.minimum(jnp.searchsorted(pad_end, jnp.arange(n_blocks, dtype=jnp.int32) * MOE_BLOCK, side='right'),
                          N_EXPERTS - 1)
    xs = h[slot_tok].reshape(n_blocks, MOE_BLOCK, D)

    def expert_block(args):
        xb, e = args
        gate, up = jnp.split(xb @ w_e1[e], 2, axis=-1)
        return (jax.nn.silu(gate) * up) @ w_e2[e]

    ys = lax.map(expert_block, (xs, block_e)).reshape(P, D)
    return jnp.zeros_like(h).at[slot_tok].add(ys * slot_w[:, None])


def setup_inputs(seed: int = 0) -> dict:
    key = jax.random.key(seed)
    ks = jax.random.split(key, 26)
    nrm = lambda k, shape, s: jax.random.normal(k, shape, F32) * s
    decay_logit = jnp.asarray(np.log(2.0 ** (5.0 + np.arange(C_HEADS)) - 1.0), F32)
    return {
        "x": nrm(ks[0], (BATCH, SEQ, D_MODEL), 1.0),
        "c": nrm(ks[1], (BATCH, D_MODEL), 1.0),
        "ctx": nrm(ks[2], (BATCH, CTX_LEN, D_MODEL), 1.0),
        "c_ctx": nrm(ks[3], (D_MODEL,), 1.0),
        "norm_mix": 1.0 + nrm(ks[4], (DEPTH, D_MODEL), 0.02),
        "norm_ffn": 1.0 + nrm(ks[5], (DEPTH, D_MODEL), 0.02),
        "w_ada": nrm(ks[6], (DEPTH, D_MODEL, 6 * D_MODEL), 0.5 * D_MODEL ** -0.5),
        "b_ada": nrm(ks[7], (DEPTH, 6 * D_MODEL), 0.02),
        "w_in": nrm(ks[8], (DEPTH, D_MODEL, IN_WIDTH), D_MODEL ** -0.5),
        "a_q_norm": 1.0 + nrm(ks[9], (DEPTH, HEAD_DIM), 0.02),
        "a_k_norm": 1.0 + nrm(ks[10], (DEPTH, HEAD_DIM), 0.02),
        "a_sink": nrm(ks[11], (DEPTH, A_Q_HEADS), 1.0),
        "b_norm": 1.0 + nrm(ks[12], (DEPTH, B_GROUPS, B_GROUP_DIM), 0.02),
        "b_spatial": nrm(ks[13], (DEPTH, B_GROUPS, B_CHUNK, B_CHUNK), B_CHUNK ** -0.5),
        "b_spatial_bias": 1.0 + nrm(ks[14], (DEPTH, B_GROUPS, B_CHUNK), 0.02),
        "c_decay_fwd": decay_logit + nrm(ks[15], (DEPTH, C_HEADS), 0.1),
        "c_decay_bwd": decay_logit + nrm(ks[16], (DEPTH, C_HEADS), 0.1),
        "c_norm": 1.0 + nrm(ks[17], (DEPTH, C_HEADS, HEAD_DIM), 0.02),
        "w_branch": nrm(ks[18], (DEPTH, N_BRANCH, BRANCH_WIDTH, D_MODEL), BRANCH_WIDTH ** -0.5),
        "w_out": nrm(ks[19], (DEPTH, D_MODEL, D_MODEL), D_MODEL ** -0.5),
        "w_router_group": nrm(ks[20], (DEPTH, D_MODEL, N_GROUPS), D_MODEL ** -0.5),
        "b_router_group": nrm(ks[21], (DEPTH, N_GROUPS), 0.01),
        "w_router_expert": nrm(ks[22], (DEPTH, D_MODEL, N_EXPERTS), D_MODEL ** -0.5),
        "b_router_expert": nrm(ks[23], (DEPTH, N_EXPERTS), 0.01),
        "w_expert_in": nrm(ks[24], (DEPTH, N_EXPERTS, D_MODEL, 2 * D_EXPERT), D_MODEL ** -0.5),
        "w_expert_out": nrm(ks[25], (DEPTH, N_EXPERTS, D_EXPERT, D_MODEL), D_EXPERT ** -0.5),
    }


def reference(x, c, ctx, c_ctx, norm_mix, norm_ffn, w_ada, b_ada, w_in, a_q_norm, a_k_norm, a_sink,
              b_norm, b_spatial, b_spatial_bias, c_decay_fwd, c_decay_bwd, c_norm, w_branch, w_out,
              w_router_group, b_router_group, w_router_expert, b_router_expert, w_expert_in, w_expert_out):
    B, N, D = x.shape
    L = ctx.shape[1]
    cos, sin = axial_rope_tables(N)
    zx, zc = x, ctx
    for l in range(DEPTH):
        last = l == DEPTH - 1
        mod_x = (jax.nn.silu(c) @ w_ada[l] + b_ada[l]).reshape(B, 6, 1, D)
        mod_c = (jax.nn.silu(c_ctx) @ w_ada[l] + b_ada[l]).reshape(6, D)
        hx = modulate(rmsnorm(zx, norm_mix[l]), mod_x[:, 0], mod_x[:, 1])
        hc = modulate(rmsnorm(zc, norm_mix[l]), mod_c[0], mod_c[1])
        mix_c, mix_x = mixing_sublayer(hc, hx, not last, cos, sin, w_in[l], a_q_norm[l], a_k_norm[l], a_sink[l],
                                       b_norm[l], b_spatial[l], b_spatial_bias[l], c_decay_fwd[l],
                                       c_decay_bwd[l], c_norm[l], w_branch[l], w_out[l])
        zx = zx + mod_x[:, 2] * mix_x
        moe_args = (w_router_group[l], b_router_group[l], w_router_expert[l], b_router_expert[l],
                    w_expert_in[l], w_expert_out[l])
        hx = modulate(rmsnorm(zx, norm_ffn[l]), mod_x[:, 3], mod_x[:, 4])
        if last:
            zx = zx + mod_x[:, 5] * moe_ffn(hx.reshape(B * N, D), *moe_args).reshape(B, N, D)
        else:
            zc = zc + mod_c[2] * mix_c
            hc = modulate(rmsnorm(zc, norm_ffn[l]), mod_c[3], mod_c[4])
            h = jnp.concatenate([hc, hx], axis=1).reshape(B * (L + N), D)
            y = moe_ffn(h, *moe_args).reshape(B, L + N, D)
            zc = zc + mod_c[5] * y[:, :L]
            zx = zx + mod_x[:, 5] * y[:, L:]
    return zx
```

```python
import contextlib
import numpy as np
import ml_dtypes
import concourse.bass as bass
import concourse.mybir as mybir
from concourse.bass_utils import run_bass_kernel_spmd

F32 = mybir.dt.float32
BF16 = mybir.dt.bfloat16
ALU = mybir.AluOpType
AF = mybir.ActivationFunctionType
AX = mybir.AxisListType
EPOCH = 30000
NCORE = 8
DEPTH = 4
D = 2048
KC = 16
LAT = 1024
CTX = 256
T = LAT + CTX
TG = [(0, 256), (256, 768), (768, 1280)]
INW = 13824
EPS = 1e-6
NEG = -30000.0


class Tile:
    def __init__(self, t, nsub=1, view=None):
        self.t = t
        self.v = view
        self.fence = None
        self.nsub = nsub
        self.lastw = [None] * nsub
        self.readers = [dict() for _ in range(nsub)]

    def __getitem__(self, idx):
        return (self.v if self.v is not None else self.t)[idx]

    def ap(self):
        return self.v if self.v is not None else self.t.ap()


class Op:
    __slots__ = ("eng", "fn", "deps", "signal", "token", "is_dma", "kind", "seq")

    def __init__(self, eng, fn, is_dma):
        self.eng = eng
        self.fn = fn
        self.deps = []
        self.signal = False
        self.token = None
        self.is_dma = is_dma
        self.kind = "dma" if is_dma else "op"


class Prog:
    ENGS = ("pe", "act", "dve", "pool", "sp")

    def __init__(self, nc, n_dma_sems=40):
        self.nc = nc
        self.st = contextlib.ExitStack()
        self.q = {e: [] for e in self.ENGS}
        self.n_dma_sems = n_dma_sems
        self.dma_rr = 0
        self.dma_last = [None] * n_dma_sems
        self.dma_cnt = [0] * n_dma_sems
        self.cc_ops = []
        self.AW = 52736
        self.arena = self.st.enter_context(nc.sbuf_tensor("arena", [128, self.AW], F32))
        self.top = 0
        self.peak = 0
        self.scopes = []
        self.fence = None
        self.dummy = self.alloc([8], F32)

    def alloc(self, shape, dtype, nsub=1):
        nelem = int(np.prod(shape))
        ncols = (nelem * (2 if dtype == BF16 else 4) + 3) // 4
        ncols = (ncols + 7) // 8 * 8
        a = self.top
        self.top += ncols
        self.peak = max(self.peak, self.top)
        assert self.top <= self.AW, ("SBUF arena overflow", self.top, shape)
        v = self.arena[:, a:a + ncols]
        if dtype == BF16:
            v = v.bitcast(BF16)
        v = v[:, 0:nelem]
        if len(shape) > 1:
            names = " ".join(f"d{i}" for i in range(len(shape)))
            v = v.rearrange(f"p ({names}) -> p {names}", **{f"d{i}": int(shape[i]) for i in range(len(shape))})
        t = Tile(None, nsub, view=v)
        t.fence = self.fence
        if self.scopes:
            self.scopes[-1][1].append(t)
        return t

    def push(self):
        self.scopes.append((self.top, []))

    def pop(self):
        top0, tiles = self.scopes.pop()
        ops = set(self.fence) if self.fence else set()
        for t in tiles:
            if t.fence:
                ops.update(t.fence)
            for s in range(t.nsub):
                if t.lastw[s] is not None:
                    ops.add(t.lastw[s])
                ops.update(t.readers[s].values())
        best = {}
        for o in ops:
            key = (o.eng,) if o.kind == "op" else (o.kind, o.token[1])
            val = o.seq if o.kind == "op" else o.token[2]
            if key not in best or best[key][0] < val:
                best[key] = (val, o)
        self.fence = frozenset(v[1] for v in best.values())
        self.top = top0

    def sbuf(self, name, shape, dtype, nsub=1):
        return Tile(self.st.enter_context(self.nc.sbuf_tensor(name, shape, dtype)), nsub)

    def psum(self, name, shape, dtype, nsub=1):
        return Tile(self.st.enter_context(self.nc.psum_tensor(name, shape, dtype)), nsub)

    def dram(self, name, shape, dtype, kind, nsub=1, **kw):
        return Tile(self.nc.dram_tensor(name, shape, dtype, kind=kind, **kw), nsub)

    @staticmethod
    def _norm(lst):
        out = []
        for r in lst:
            if isinstance(r, Tile):
                out.append((r, range(r.nsub)))
            else:
                t, s = r
                if s is None:
                    out.append((t, range(t.nsub)))
                elif isinstance(s, int):
                    out.append((t, (s,)))
                else:
                    out.append((t, s))
        return out

    def add(self, eng, fn, reads=(), writes=(), is_dma=False, cc=False):
        op = Op(eng, fn, is_dma)
        deps = set()
        reads = self._norm(reads)
        writes = self._norm(writes)
        for t, subs in reads:
            for s in subs:
                if t.lastw[s] is not None:
                    deps.add(t.lastw[s])
                elif t.fence:
                    deps.update(t.fence)
        for t, subs in writes:
            for s in subs:
                if t.lastw[s] is not None:
                    deps.add(t.lastw[s])
                elif t.fence:
                    deps.update(t.fence)
                for r in t.readers[s].values():
                    deps.add(r)
        async_ = is_dma or cc
        for t, subs in reads:
            for s in subs:
                key = ("a", id(op)) if async_ else eng
                t.readers[s][key] = op
        for t, subs in writes:
            for s in subs:
                t.lastw[s] = op
                t.readers[s] = dict()
        if is_dma:
            k = self.dma_rr
            self.dma_rr = (k + 1) % self.n_dma_sems
            if self.dma_last[k] is not None:
                deps.add(self.dma_last[k])
            self.dma_last[k] = op
            self.dma_cnt[k] += 16
            op.token = ("dma", k, self.dma_cnt[k])
        if cc:
            op.kind = "cc"
            self.cc_ops.append(op)
            op.token = ("cc", len(self.cc_ops) - 1, 1)
        deps.discard(op)
        for d in deps:
            if d.eng == "pe" and eng == "pe" and d.kind == "op" and op.kind == "op":
                continue
            d.signal = True
            op.deps.append(d)
        op.seq = len(self.q[eng])
        self.q[eng].append(op)
        return op

    def dma(self, eng, out, in_, reads=(), writes=(), **kw):
        return self.add(eng, lambda e: e.dma_start(out=out, in_=in_, **kw), reads, writes, is_dma=True)

    def mm(self, out, lhsT, rhs, start, stop, reads, writes):
        return self.add("pe", lambda e: e.matmul(out, lhsT, rhs, start=start, stop=stop), reads, writes)

    def tr(self, out, in_, ident, reads, writes):
        return self.add("pe", lambda e: e.transpose(out, in_, ident), reads, writes)

    def act(self, out, in_, func, reads, writes, **kw):
        return self.add("act", lambda e: e.activation(out=out, in_=in_, func=func, **kw), reads, writes)

    def tt(self, eng, out, in0, in1, op, reads, writes):
        return self.add(eng, lambda e: e.tensor_tensor(out=out, in0=in0, in1=in1, op=op), reads, writes)

    def ts(self, eng, out, in0, s1, s2, op0, op1, reads, writes):
        if op1 is None:
            return self.add(eng, lambda e: e.tensor_scalar(out=out, in0=in0, scalar1=s1, scalar2=None, op0=op0), reads, writes)
        return self.add(eng, lambda e: e.tensor_scalar(out=out, in0=in0, scalar1=s1, scalar2=s2, op0=op0, op1=op1), reads, writes)

    def stt(self, eng, out, in0, scalar, in1, op0, op1, reads, writes):
        return self.add(eng, lambda e: e.scalar_tensor_tensor(out=out, in0=in0, scalar=scalar, in1=in1, op0=op0, op1=op1), reads, writes)

    def cp(self, eng, out, in_, reads, writes):
        if eng == "act":
            return self.add(eng, lambda e: e.copy(out=out, in_=in_), reads, writes)
        return self.add(eng, lambda e: e.tensor_copy(out=out, in_=in_), reads, writes)

    def memset(self, eng, ap, val, writes):
        return self.add(eng, lambda e: e.memset(ap, val), (), writes)

    def emit(self, final_wait_ops=()):
        nc = self.nc
        nsem = {}
        for e in self.ENGS:
            c = 0
            for op in self.q[e]:
                if op.kind != "op":
                    continue
                if op.signal:
                    c += 1
                    op.token = (e, (c - 1) // EPOCH, (c - 1) % EPOCH + 1)
            nsem[e] = (c - 1) // EPOCH + 1 if c else 0
        sems = {}
        for e in self.ENGS:
            for k in range(nsem[e]):
                sems[(e, k)] = self.st.enter_context(nc.semaphore(f"s_{e}_{k}"))
        for k in range(self.n_dma_sems):
            sems[("dma", k)] = self.st.enter_context(nc.semaphore(f"s_dma_{k}"))
        for k in range(len(self.cc_ops)):
            sems[("cc", k)] = self.st.enter_context(nc.semaphore(f"s_cc_{k}"))
        block = self.st.enter_context(nc.Block())
        hw = {"pe": block.tensor, "act": block.scalar, "dve": block.vector, "pool": block.gpsimd, "sp": block.sync}

        def make(e):
            ops = self.q[e]

            def body(eng):
                waited = {}
                for op in ops:
                    need = {}
                    for d in op.deps:
                        key = (d.token[0], d.token[1])
                        if need.get(key, 0) < d.token[2]:
                            need[key] = d.token[2]
                    for key, v in need.items():
                        if waited.get(key, 0) >= v:
                            continue
                        eng.wait_ge(sems[key], v)
                        waited[key] = v
                    inst = op.fn(eng)
                    if op.kind == "dma":
                        inst.then_inc(sems[(op.token[0], op.token[1])], 16)
                    elif op.kind == "cc":
                        inst.then_inc(sems[(op.token[0], op.token[1])], 1)
                    elif op.signal:
                        inst.then_inc(sems[(op.token[0], op.token[1])], 1)
                if e == "sp":
                    for op in final_wait_ops:
                        eng.wait_ge(sems[(op.token[0], op.token[1])], op.token[2])

            return body

        for e in self.ENGS:
            hw[e](make(e))

    def close(self):
        self.st.close()


def _mk(spec):
    d, o = {}, 0
    for n, w in spec:
        d[n] = (o, o + w)
        o += w
    return d, o


PK, NPK = _mk((("norm_mix", 16), ("norm_ffn", 16), ("b_ada", 96), ("a_q_norm", 1), ("a_k_norm", 1), ("c_norm", 8),
               ("a_sink", 8), ("c_decay", 16), ("r_bias", 36)))
CK, NCK = _mk((("cj", 2), ("coefn", 18), ("coefv", 18), ("hsel", 16), ("silu_in", 32), ("eps", 1)))
CB, NCB = _mk((("ident", 128), ("ones", 128), ("perm", 128)))
RC, NRC = _mk((("D1", 128), ("U1", 128), ("D2", 128), ("U2", 128), ("I1", 128), ("I2", 128)))
MK, NMK = _mk((("mL0", 512), ("mL", 512), ("mR", 512), ("mR7", 512)))

WSPEC = {
    "w_ada": (2048, 12288), "w_in": (2048, INW), "w_branch": (3072, 2048), "w_out": (2048, 2048),
    "w_e1": (32 * 2048, 1024), "w_e2": (32 * 512, 2048),
}
XPW = 3072


def build_program(depth=DEPTH, n_experts=32, debug=False):
    nc = bass.Bass("TRN2", target_bir_lowering=False)
    P = Prog(nc)

    z0 = P.dram("z0", [KC, 128, T], F32, "ExternalInput")
    pk_d = P.dram("pk", [depth, 128, NPK], F32, "ExternalInput")
    ck_d = P.dram("ck", [128, NCK], F32, "ExternalInput")
    cb_d = P.dram("cb", [128, NCB], F32, "ExternalInput")
    rc_d = P.dram("rc", [128, NRC], F32, "ExternalInput")
    mk_d = P.dram("mk", [128, NMK], F32, "ExternalInput")
    rope_d = P.dram("rope", [128, 2048], F32, "ExternalInput")
    sel_d = P.dram("sel", [32, 4096], F32, "ExternalInput")
    bsp_d = P.dram("bsp", [depth, 128, 1024], F32, "ExternalInput")
    pk2_d = P.dram("pk2", [depth, 128, 2048], F32, "ExternalInput")
    wr_d = P.dram("wr", [depth, 128, KC * 36], F32, "ExternalInput")
    out_d = P.dram("out", [KC, 128, LAT], F32, "ExternalOutput")
    wsh, wint, wfull = {}, {}, {}
    for n, (r, c) in WSPEC.items():
        wsh[n] = P.dram(n + "_s", [depth, r // NCORE, c], F32, "ExternalInput")
        wint[n] = [P.dram(f"{n}_i{l}", [r // NCORE, c], F32, "Internal") for l in range(2)]
        wfull[n] = [P.dram(f"{n}_f{l}", [r, c], F32, "Internal", addr_space="Shared") for l in range(2)]
    z_d = P.dram("z_d", [KC, 128, T], F32, "Internal", nsub=KC * 3)
    br_d = P.dram("br_d", [24, 128, T], BF16, "Internal", nsub=24)
    aq_d = P.dram("aq_d", [8, 128, T], BF16, "Internal", nsub=8)
    rq_d = P.dram("rq_d", [8, 128, T], BF16, "Internal", nsub=8)
    rk_d = P.dram("rk_d", [8, 128, T], BF16, "Internal", nsub=8)
    rg_d = P.dram("rg_d", [8, 128, T], BF16, "Internal", nsub=8)
    bu_d = P.dram("bu_d", [8, 128, T], BF16, "Internal", nsub=8)
    rkt_d = P.dram("rkt_d", [10, 128, 1024], BF16, "Internal", nsub=10)
    rv_d = P.dram("rv_d", [10, 128, 1024], BF16, "Internal", nsub=10)
    bv_d = P.dram("bv_d", [10, 128, 1024], BF16, "Internal", nsub=10)
    xin_d = [P.dram(f"xin{l}", [128, XPW], F32, "Internal") for l in range(depth)]
    xout_d = [P.dram(f"xout{l}", [NCORE * 128, XPW], F32, "Internal", addr_space="Shared") for l in range(depth)]
    dbg = {}

    def zs(kc, g=None):
        return (z_d, range(kc * 3, kc * 3 + 3)) if g is None else (z_d, kc * 3 + g)

    ck = P.alloc([NCK], F32)
    cb = P.alloc([NCB], BF16)
    pkl = P.alloc([NPK], F32)
    hT = P.alloc([KC, T], BF16, nsub=KC)
    ring = [P.alloc([4096], BF16) for _ in range(5)]
    modT = P.alloc([96, 2], F32)
    gm = P.alloc([2, KC, 2], F32)
    scb = P.alloc([KC, 2], BF16)
    rix = [0]

    def slot():
        t = ring[rix[0] % len(ring)]
        rix[0] += 1
        return t

    pbs = [P.psum(f"pb{i}", [128, 512], F32) for i in range(7)]
    pbt = P.psum("pbt", [128, 1024], BF16)
    pix = [0]

    def pb():
        t = pbs[pix[0] % len(pbs)]
        pix[0] += 1
        return t

    def C(name):
        a, b = CK[name]
        return ck[:, a:b]

    def B(name):
        a, b = CB[name]
        return cb[:, a:b]

    def PKc(name):
        a, b = PK[name]
        return pkl[:, a:b]

    ident, ones, perm = B("ident"), B("ones"), B("perm")
    epsc = C("eps")
    P.dma("sp", ck[:], ck_d[:, :], writes=[ck])
    P.dma("pool", cb[:], cb_d[:, :], writes=[cb])

    AG = lambda ia, oa: (lambda e: e.collective_compute("AllGather", ALU.bypass, replica_groups=[list(range(NCORE))], ins=[ia], outs=[oa]))

    def gather_layer(l):
        for n in ("w_ada", "w_in", "w_branch", "w_out", "w_e1", "w_e2"):
            r, c = WSPEC[n]
            rs = r // NCORE
            step = max(1, (4 << 20) // (c * 4))
            for r0 in range(0, rs, step):
                r1 = min(rs, r0 + step)
                P.dma("sp", wint[n][l % 2].ap()[r0:r1, :], wsh[n].ap()[l, r0:r1, :], writes=[wint[n][l % 2]])
            P.add("pool", AG(wint[n][l % 2].ap(), wfull[n][l % 2].ap()), reads=[wint[n][l % 2]], writes=[wfull[n][l % 2]], cc=True)

    gather_layer(0)
    if depth > 1:
        gather_layer(1)

    for kc in range(KC):
        P.dma("sp", z_d[kc], z0[kc], writes=[zs(kc)])
    P.act(scb[:].rearrange("p k w -> p (k w)"), C("silu_in"), AF.Silu, reads=[ck], writes=[scb])

    WHICH = ((0, CTX, 1), (CTX, T, 0))
    TGW = (1, 0, 0)

    def load_w(dst_ap, src_ap, dst_tile, src_tile):
        P.dma("pool", dst_ap, src_ap, reads=[src_tile], writes=[dst_tile])

    for l in range(depth):
        if l >= 1 and l + 1 < depth:
            gather_layer(l + 1)
        P.dma("sp", pkl[:], pk_d[l], writes=[pkl])

        wada = wfull["w_ada"][l % 2]
        pm = pb()
        for cg in range(48):
            sl = slot()
            slv = sl[:].rearrange("p (k c) -> p k c", k=KC)
            load_w(slv, wada.ap()[:, cg * 256:(cg + 1) * 256].rearrange("(k p) c -> p k c", p=128), sl, wada)
            for h in range(2):
                cc_ = cg * 2 + h
                for kc in range(KC):
                    P.mm(pm[:, cc_ * 2:cc_ * 2 + 2], slv[:, kc, h * 128:(h + 1) * 128], scb[:, kc, :], kc == 0, kc == KC - 1, reads=[sl, scb], writes=[pm])
        P.tt("dve", modT[:], pm[:, 0:192].rearrange("p (c w) -> p c w", w=2), PKc("b_ada").unsqueeze(2).to_broadcast([128, 96, 2]), ALU.add, reads=[pm, pkl], writes=[modT])
        for s, (nn, mi) in enumerate((("norm_mix", 1), ("norm_ffn", 4))):
            P.stt("dve", gm[:, s], modT[:, mi * 16:(mi + 1) * 16, :], 1.0, PKc(nn).unsqueeze(2).to_broadcast([128, KC, 2]), ALU.add, ALU.mult, reads=[modT, pkl], writes=[gm])

        def make_hT(s, shift_m):
            P.push()
            zb = [P.alloc([T], F32) for _ in range(2)]
            sq = [P.alloc([T], BF16) for _ in range(2)]
            rstd = P.alloc([T], F32)
            tmp = [P.alloc([T], F32) for _ in range(2)]
            pss = [pb() for _ in range(3)]
            for kc in range(KC):
                zt = zb[kc % 2]
                P.dma("sp", zt[:], z_d[kc], reads=[zs(kc)], writes=[zt])
                sqt = sq[kc % 2]
                P.act(sqt[:], zt[:], AF.Square, reads=[zt], writes=[sqt])
                for g, (lo, hi) in enumerate(TG):
                    P.mm(pss[g][:, 0:hi - lo], ones, sqt[:, lo:hi], kc == 0, kc == KC - 1, reads=[sqt, cb], writes=[pss[g]])
            for g, (lo, hi) in enumerate(TG):
                P.act(rstd[:, lo:hi], pss[g][:, 0:hi - lo], AF.Sqrt, reads=[pss[g], ck], writes=[rstd], bias=epsc, scale=1.0 / D)
            P.add("dve", lambda e: e.reciprocal(out=rstd[:], in_=rstd[:]), reads=[rstd], writes=[rstd])
            for kc in range(KC):
                zt = zb[kc % 2]
                P.dma("sp", zt[:], z_d[kc], reads=[zs(kc)], writes=[zt])
                tp = tmp[kc % 2]
                for lo, hi, w in WHICH:
                    P.stt("dve", tp[:, lo:hi], zt[:, lo:hi], gm[:, s, kc, w:w + 1], rstd[:, lo:hi], ALU.mult, ALU.mult, reads=[zt, gm, rstd], writes=[tp])
                    P.act(hT[:, kc, lo:hi], tp[:, lo:hi], AF.Identity, reads=[tp, modT], writes=[(hT, kc)], bias=modT[:, shift_m * 16 + kc, w:w + 1], scale=1.0)
            P.pop()

        make_hT(0, 0)
        if debug and l == 0:
            dbg["hT"] = P.dram("dbg_hT", [128, KC * T], BF16, "ExternalOutput")
            dbg["hT_op"] = P.dma("sp", dbg["hT"].ap(), hT[:].rearrange("p k t -> p (k t)"), reads=[hT], writes=[dbg["hT"]])

        win = wfull["w_in"][l % 2]

        def load_in_cols(c0):
            sl = slot()
            slv = sl[:].rearrange("p (k c) -> p k c", k=KC)
            load_w(slv, win.ap()[:, c0:c0 + 256].rearrange("(k p) c -> p k c", p=128), sl, win)
            return sl, slv

        def proj_fm(sl, slv, h, g):
            lo, hi = TG[g]
            ps = pb()
            for kc in range(KC):
                P.mm(ps[:, 0:hi - lo], slv[:, kc, h * 128:(h + 1) * 128], hT[:, kc, lo:hi], kc == 0, kc == KC - 1, reads=[sl, (hT, kc)], writes=[ps])
            return ps

        def proj_tm(sl, slv, c):
            ps = pb()
            for kc in range(KC):
                P.mm(ps[:, 0:256], hT[:, kc, c * 128:(c + 1) * 128], slv[:, kc, :], kc == 0, kc == KC - 1, reads=[sl, (hT, kc)], writes=[ps])
            return ps

        P.push()
        kTa = P.alloc([2, 1536], BF16)
        va = P.alloc([12, 256], BF16)
        rc = P.alloc([NRC], F32)
        lgb = P.alloc([16], F32)
        MT = P.alloc([8, 128], F32)
        AFB = P.alloc([2, 8, 128], F32)
        KD = P.alloc([2, 8], F32)
        GD = P.alloc([2, 8], F32)
        coef = P.alloc([2, 9, 8], F32)
        sinkexp = P.alloc([8], F32)
        kv_p = [(P.alloc([8, 128], BF16), P.alloc([8, 128], BF16)) for _ in range(2)]
        kd_p = [P.alloc([8, 128], BF16) for _ in range(2)]
        Scf = P.alloc([8, 128], F32)
        Scb = P.alloc([8, 128], F32)
        SstF = P.alloc([8, 128], F32)
        SstB = P.alloc([8, 128], F32)
        P.push()
        rope = P.alloc([2048], F32)
        P.dma("sp", rope[:], rope_d[:, :], writes=[rope])
        cosT, sinT = rope[:, 0:1024], rope[:, 1024:2048]
        NR = 2
        xn_p = [P.alloc([512], F32) for _ in range(NR)]
        sqh_p = [P.alloc([512], BF16) for _ in range(NR)]
        rs_p = [P.alloc([512], F32) for _ in range(NR)]
        xb_p = [P.alloc([512], BF16) for _ in range(NR)]
        t1_p = [P.alloc([512], F32) for _ in range(NR)]
        nrc = [0]

        def norm_rope(ps, g, dst_ap, dst_dep, norm_col, do_norm, post_scale=1.0):
            lo, hi = TG[g]
            n = hi - lo
            i = nrc[0] % NR
            nrc[0] += 1
            xn, sqh, rs, xb, t1 = xn_p[i], sqh_p[i], rs_p[i], xb_p[i], t1_p[i]
            if do_norm:
                P.act(sqh[:, 0:n], ps[:, 0:n], AF.Square, reads=[ps], writes=[sqh])
                p2 = pb()
                P.mm(p2[:, 0:n], ones, sqh[:, 0:n], True, True, reads=[sqh, cb], writes=[p2])
                P.act(rs[:, 0:n], p2[:, 0:n], AF.Sqrt, reads=[p2, ck], writes=[rs], bias=epsc, scale=1.0 / 128)
                P.add("dve", lambda e: e.reciprocal(out=rs[:, 0:n], in_=rs[:, 0:n]), reads=[rs], writes=[rs])
                P.stt("dve", xn[:, 0:n], ps[:, 0:n], norm_col, rs[:, 0:n], ALU.mult, ALU.mult, reads=[ps, rs, pkl], writes=[xn])
            else:
                P.act(xn[:, 0:n], ps[:, 0:n], AF.Copy, reads=[ps], writes=[xn], scale=post_scale)
            if g == 0:
                P.cp("pool", dst_ap, xn[:, 0:n], reads=[xn], writes=[dst_dep])
                return
            P.cp("act", xb[:, 0:n], xn[:, 0:n], reads=[xn], writes=[xb])
            p3 = pb()
            P.mm(p3[:, 0:n], perm, xb[:, 0:n], True, True, reads=[xb, cb], writes=[p3])
            P.tt("dve", t1[:, 0:n], p3[:, 0:n], sinT[:, lo - CTX:hi - CTX], ALU.mult, reads=[p3, rope], writes=[t1])
            P.tt("pool", xn[:, 0:n], xn[:, 0:n], cosT[:, lo - CTX:hi - CTX], ALU.mult, reads=[xn, rope], writes=[xn])
            P.tt("dve", dst_ap, xn[:, 0:n], t1[:, 0:n], ALU.add, reads=[xn, t1], writes=[dst_dep])

        a_k = PKc("a_k_norm")
        a_q = PKc("a_q_norm")

        def kcol(g):
            lo, hi = TG[g]
            return (lo, hi) if g == 0 else (lo + 128, hi + 128)

        hb_p = [P.alloc([T], BF16) for _ in range(3)]
        hbc = [0]

        def headbuf():
            t = hb_p[hbc[0] % 3]
            hbc[0] += 1
            return t

        sl, slv = load_in_cols(0)
        for h in range(2):
            for g in range(3):
                ps = proj_fm(sl, slv, h, g)
                a, b = kcol(g)
                norm_rope(ps, g, kTa[:, h, a:b], kTa, a_k, True)
        sl, slv = load_in_cols(256)
        for c in range(10):
            ps = proj_tm(sl, slv, c)
            ci = c if c < 2 else c + 1
            P.cp("act", va[:, ci, :], ps[:, 0:256], reads=[ps], writes=[va])
        kscale = 128 ** -0.5
        for hp in range(4):
            sl, slv = load_in_cols(512 + hp * 256)
            for hh in range(2):
                h = hp * 2 + hh
                kh = headbuf()
                for g in range(3):
                    ps = proj_fm(sl, slv, hh, g)
                    lo, hi = TG[g]
                    norm_rope(ps, g, kh[:, lo:hi], kh, None, False, post_scale=kscale)
                P.dma("sp", rk_d[h], kh[:], reads=[kh], writes=[(rk_d, h)])
        kct_p = [P.alloc([8, 128], BF16) for _ in range(2)]
        ktk_p = [P.alloc([1024], BF16) for _ in range(2)]
        for c in range(10):
            kc_t = kct_p[c % 2]
            P.dma("sp", kc_t[:], rk_d.ap()[:, :, c * 128:(c + 1) * 128].rearrange("h p t -> p h t"), reads=[rk_d], writes=[kc_t])
            for h in range(8):
                P.tr(pbt[:, h * 128:(h + 1) * 128], kc_t[:, h, :], ident, reads=[kc_t, cb], writes=[pbt])
            kt = ktk_p[c % 2]
            P.cp("act", kt[:], pbt[:], reads=[pbt], writes=[kt])
            P.dma("sp", rkt_d[c], kt[:], reads=[kt], writes=[(rkt_d, c)])
        vt_p = [P.alloc([256], BF16) for _ in range(3)]
        for cq in range(4):
            sl, slv = load_in_cols(1536 + cq * 256)
            for c in range(10):
                ps = proj_tm(sl, slv, c)
                vt = vt_p[c % 3]
                P.cp("act", vt[:], ps[:, 0:256], reads=[ps], writes=[vt])
                P.dma("sp", rv_d.ap()[c, :, cq * 256:(cq + 1) * 256], vt[:], reads=[vt], writes=[(rv_d, c)])

        P.dma("sp", rc[:], rc_d[:, :], writes=[rc])

        def R(name):
            a, b = RC[name]
            return rc[:, a:b]

        P.act(lgb[:], PKc("c_decay"), AF.Exp, reads=[pkl], writes=[lgb], scale=-1.0)
        P.ts("dve", lgb[:], lgb[:], 1.0, None, ALU.add, None, reads=[lgb], writes=[lgb])
        P.act(lgb[:], lgb[:], AF.Ln, reads=[lgb], writes=[lgb])
        P.ts("dve", lgb[:], lgb[:], -1.0, None, ALU.mult, None, reads=[lgb], writes=[lgb])
        tmpm = P.alloc([128], F32)
        for h in range(8):
            P.act(MT[:, h, :], R("D1"), AF.Exp, reads=[rc, lgb], writes=[MT], scale=lgb[:, h:h + 1])
            P.tt("dve", MT[:, h, :], MT[:, h, :], R("U1"), ALU.mult, reads=[MT, rc], writes=[MT])
            P.act(tmpm[:], R("D2"), AF.Exp, reads=[rc, lgb], writes=[tmpm], scale=lgb[:, 8 + h:9 + h])
            P.tt("dve", tmpm[:], tmpm[:], R("U2"), ALU.mult, reads=[tmpm, rc], writes=[tmpm])
            P.tt("dve", MT[:, h, :], MT[:, h, :], tmpm[:], ALU.add, reads=[MT, tmpm], writes=[MT])
            P.act(AFB[:, 0, h, :], R("I1"), AF.Exp, reads=[rc, lgb], writes=[AFB], scale=lgb[:, h:h + 1])
            P.act(AFB[:, 1, h, :], R("I2"), AF.Exp, reads=[rc, lgb], writes=[AFB], scale=lgb[:, 8 + h:9 + h])
        cj = C("cj")
        for dr in range(2):
            P.ts("dve", KD[:, dr, :], lgb[:, dr * 8:dr * 8 + 8], cj[:, dr:dr + 1], None, ALU.mult, None, reads=[lgb, ck], writes=[KD])
            P.ts("dve", GD[:, dr, :], lgb[:, dr * 8:dr * 8 + 8], 128.0, None, ALU.mult, None, reads=[lgb], writes=[GD])
        P.act(KD[:], KD[:], AF.Exp, reads=[KD], writes=[KD])
        P.act(GD[:], GD[:], AF.Exp, reads=[GD], writes=[GD])
        cn, cv = C("coefn"), C("coefv")
        for dr in range(2):
            for s in range(9):
                P.ts("dve", coef[:, dr, s, :], lgb[:, dr * 8:dr * 8 + 8], cn[:, dr * 9 + s:dr * 9 + s + 1], None, ALU.mult, None, reads=[lgb, ck], writes=[coef])
        P.act(coef[:], coef[:], AF.Exp, reads=[coef], writes=[coef])
        for dr in range(2):
            P.tt("dve", coef[:, dr], coef[:, dr], cv[:, dr * 9:dr * 9 + 9].unsqueeze(2).to_broadcast([128, 9, 8]), ALU.mult, reads=[coef, ck], writes=[coef])
        P.act(sinkexp[:], PKc("a_sink"), AF.Exp, reads=[pkl], writes=[sinkexp])

        kvc = [0]

        def load_kv_chunk(c):
            kt, vt = kv_p[kvc[0] % 2]
            kvc[0] += 1
            P.dma("sp", kt[:].rearrange("p h d -> p (h d)"), rkt_d[c], reads=[(rkt_d, c)], writes=[kt])
            P.dma("sp", vt[:].rearrange("p h d -> p (h d)"), rv_d[c], reads=[(rv_d, c)], writes=[vt])
            return kt, vt

        kdc = [0]

        def state_step(S, kt, vt, dr):
            kd = kd_p[kdc[0] % 2]
            kdc[0] += 1
            P.tt("pool", kd[:], kt[:], KD[:, dr, :].unsqueeze(2).to_broadcast([128, 8, 128]), ALU.mult, reads=[kt, KD], writes=[kd])
            pa, pb2 = pb(), pb()
            for h in range(8):
                pp = pa if h < 4 else pb2
                P.mm(pp[:, (h % 4) * 128:(h % 4 + 1) * 128], kd[:, h, :], vt[:, h, :], True, True, reads=[kd, vt], writes=[pp])
            P.tt("dve", S[:], S[:], GD[:, dr, :].unsqueeze(2).to_broadcast([128, 8, 128]), ALU.mult, reads=[S, GD], writes=[S])
            P.tt("dve", S[:, 0:4, :], S[:, 0:4, :], pa[:].rearrange("p (h e) -> p h e", h=4), ALU.add, reads=[S, pa], writes=[S])
            P.tt("dve", S[:, 4:8, :], S[:, 4:8, :], pb2[:].rearrange("p (h e) -> p h e", h=4), ALU.add, reads=[S, pb2], writes=[S])

        xst = P.alloc([XPW], F32)
        Sf = xst[:, 1024:2048].rearrange("p (h e) -> p h e", h=8)
        Sb = xst[:, 2048:3072].rearrange("p (h e) -> p h e", h=8)

        class V:
            def __init__(s, tile, v):
                s.tile, s.v = tile, v

        P.memset("pool", xst[:], 0.0, writes=[xst])
        P.memset("pool", Scf[:], 0.0, writes=[Scf])
        P.memset("pool", Scb[:], 0.0, writes=[Scb])

        def state_step_v(Sv, Stile, kt, vt, dr):
            kd = kd_p[kdc[0] % 2]
            kdc[0] += 1
            P.tt("pool", kd[:], kt[:], KD[:, dr, :].unsqueeze(2).to_broadcast([128, 8, 128]), ALU.mult, reads=[kt, KD], writes=[kd])
            pa, pb2 = pb(), pb()
            for h in range(8):
                pp = pa if h < 4 else pb2
                P.mm(pp[:, (h % 4) * 128:(h % 4 + 1) * 128], kd[:, h, :], vt[:, h, :], True, True, reads=[kd, vt], writes=[pp])
            P.tt("dve", Sv, Sv, GD[:, dr, :].unsqueeze(2).to_broadcast([128, 8, 128]), ALU.mult, reads=[Stile, GD], writes=[Stile])
            P.tt("dve", Sv[:, 0:4, :], Sv[:, 0:4, :], pa[:].rearrange("p (h e) -> p h e", h=4), ALU.add, reads=[Stile, pa], writes=[Stile])
            P.tt("dve", Sv[:, 4:8, :], Sv[:, 4:8, :], pb2[:].rearrange("p (h e) -> p h e", h=4), ALU.add, reads=[Stile, pb2], writes=[Stile])

        for c in range(2, 10):
            kt, vt = load_kv_chunk(c)
            state_step_v(Sf, xst, kt, vt, 0)
        for c in range(9, 1, -1):
            kt, vt = load_kv_chunk(c)
            state_step_v(Sb, xst, kt, vt, 1)
        for c in (0, 1):
            kt, vt = load_kv_chunk(c)
            state_step_v(Scf[:], Scf, kt, vt, 0)
        for c in (1, 0):
            kt, vt = load_kv_chunk(c)
            state_step_v(Scb[:], Scb, kt, vt, 1)

        xk = xst[:, 0:512].rearrange("p (h w c) -> p h w c", h=2, w=2)
        for h in range(2):
            P.cp("act", xk[:, h, 0, :], kTa[:, h, 384:512], reads=[kTa], writes=[xst])
            P.cp("act", xk[:, h, 1, :], kTa[:, h, 384 + 896:384 + 1024], reads=[kTa], writes=[xst])
        xv = xst[:, 512:1024].rearrange("p (w c) -> p w c", w=2)
        P.cp("act", xv[:, 0, :], va[:, 3, :], reads=[va], writes=[xst])
        P.cp("act", xv[:, 1, :], va[:, 10, :], reads=[va], writes=[xst])
        xin, xout = xin_d[l], xout_d[l]
        P.dma("sp", xin.ap(), xst[:], reads=[xst], writes=[xin])
        P.add("pool", AG(xin.ap(), xout.ap()), reads=[xin], writes=[xout], cc=True)

        for hp in range(4):
            sl, slv = load_in_cols(2560 + hp * 256)
            for hh in range(2):
                h = hp * 2 + hh
                qh = headbuf()
                for g in range(3):
                    ps = proj_fm(sl, slv, hh, g)
                    lo, hi = TG[g]
                    norm_rope(ps, g, qh[:, lo:hi], qh, a_q, True)
                P.dma("sp", aq_d[h], qh[:], reads=[qh], writes=[(aq_d, h)])
        for hp in range(4):
            sl, slv = load_in_cols(3584 + hp * 256)
            for hh in range(2):
                h = hp * 2 + hh
                qh = headbuf()
                for g in range(3):
                    ps = proj_fm(sl, slv, hh, g)
                    lo, hi = TG[g]
                    norm_rope(ps, g, qh[:, lo:hi], qh, None, False)
                P.dma("sp", rq_d[h], qh[:], reads=[qh], writes=[(rq_d, h)])
        for seg, fn, dst in ((4608, AF.Silu, rg_d), (5632, AF.Gelu_apprx_tanh, bu_d)):
            for hp in range(4):
                sl, slv = load_in_cols(seg + hp * 256)
                for hh in range(2):
                    h = hp * 2 + hh
                    gh = headbuf()
                    for g in range(3):
                        ps = proj_fm(sl, slv, hh, g)
                        lo, hi = TG[g]
                        P.act(gh[:, lo:hi], ps[:, 0:hi - lo], fn, reads=[ps], writes=[gh])
                    P.dma("sp", dst[h], gh[:], reads=[gh], writes=[(dst, h)])
        bnorm_t = P.alloc([1024], F32)
        P.dma("sp", bnorm_t[:], pk2_d.ap()[l, :, 1024:2048], writes=[bnorm_t])
        bnorm = bnorm_t[:]
        gv_p = [P.alloc([2, 128], F32) for _ in range(2)]
        sq2_p = [P.alloc([2, 128], F32) for _ in range(2)]
        st_p = [P.alloc([2], F32) for _ in range(2)]
        vb_p = [P.alloc([256], BF16) for _ in range(2)]
        for cq in range(4):
            sl, slv = load_in_cols(6656 + cq * 256)
            for c in range(10):
                ps = proj_tm(sl, slv, c)
                gv, sq2, st, vb = gv_p[c % 2], sq2_p[c % 2], st_p[c % 2], vb_p[c % 2]
                P.act(gv[:].rearrange("p a d -> p (a d)"), ps[:, 0:256], AF.Gelu_apprx_tanh, reads=[ps], writes=[gv])
                P.add("dve", lambda e, st=st, gv=gv: e.tensor_reduce(out=st[:], in_=gv[:], axis=AX.X, op=ALU.add), reads=[gv], writes=[st])
                P.ts("dve", st[:], st[:], -1.0 / 128, None, ALU.mult, None, reads=[st], writes=[st])
                P.tt("dve", gv[:], gv[:], st[:].unsqueeze(2).to_broadcast([128, 2, 128]), ALU.add, reads=[gv, st], writes=[gv])
                P.tt("pool", sq2[:], gv[:], gv[:], ALU.mult, reads=[gv], writes=[sq2])
                P.add("dve", lambda e, st=st, sq2=sq2: e.tensor_reduce(out=st[:], in_=sq2[:], axis=AX.X, op=ALU.add), reads=[sq2], writes=[st])
                P.act(st[:], st[:], AF.Sqrt, reads=[st, ck], writes=[st], bias=epsc, scale=1.0 / 128)
                P.add("dve", lambda e, st=st: e.reciprocal(out=st[:], in_=st[:]), reads=[st], writes=[st])
                P.tt("dve", gv[:], gv[:], st[:].unsqueeze(2).to_broadcast([128, 2, 128]), ALU.mult, reads=[gv, st], writes=[gv])
                P.tt("pool", vb[:], gv[:].rearrange("p a d -> p (a d)"), bnorm[:, cq * 256:(cq + 1) * 256], ALU.mult, reads=[gv, bnorm_t], writes=[vb])
                P.dma("sp", bv_d.ap()[c, :, cq * 256:(cq + 1) * 256], vb[:], reads=[vb], writes=[(bv_d, c)])

        P.pop()
        P.push()
        wsT = P.alloc([8, 128], BF16)
        P.dma("pool", wsT[:].rearrange("p g q -> p (g q)"), bsp_d[l], writes=[wsT])
        bbias_t = P.alloc([1024], F32)
        P.dma("sp", bbias_t[:], pk2_d.ap()[l, :, 0:1024], writes=[bbias_t])
        bbias = bbias_t[:]
        vh_p = [P.alloc([1024], BF16) for _ in range(2)]
        u_p = [P.alloc([8, 128], BF16) for _ in range(2)]
        tg_p = [P.alloc([8, 128], F32) for _ in range(2)]
        go_p = [P.alloc([8, 128], BF16) for _ in range(2)]
        for c in range(10):
            vh, uc, tg_, go = vh_p[c % 2], u_p[c % 2], tg_p[c % 2], go_p[c % 2]
            P.dma("sp", vh[:], bv_d[c], reads=[(bv_d, c)], writes=[vh])
            P.dma("sp", uc[:], bu_d.ap()[:, :, c * 128:(c + 1) * 128].rearrange("h p t -> p h t"), reads=[bu_d], writes=[uc])
            pa, pb2 = pb(), pb()
            for g in range(8):
                pp = pa if g < 4 else pb2
                P.mm(pp[:, (g % 4) * 128:(g % 4 + 1) * 128], vh[:, g * 128:(g + 1) * 128], wsT[:, g, :], True, True, reads=[vh, wsT], writes=[pp])
            for half, pp in enumerate((pa, pb2)):
                P.tt("dve", tg_[:, half * 4:half * 4 + 4, :], pp[:].rearrange("p (g q) -> p g q", g=4), bbias[:, half * 512:(half + 1) * 512].rearrange("p (g q) -> p g q", g=4), ALU.add, reads=[pp, bbias_t], writes=[tg_])
            P.tt("pool", go[:], tg_[:], uc[:], ALU.mult, reads=[tg_, uc], writes=[go])
            P.dma("sp", br_d.ap()[8:16, :, c * 128:(c + 1) * 128].rearrange("h p t -> p h t"), go[:], reads=[go], writes=[(br_d, range(8, 16))])
        P.pop()

        P.push()
        xs_p = [P.alloc([XPW], F32) for _ in range(2)]
        hk = P.alloc([2, 2, 128], F32)
        hv = P.alloc([2, 256], F32)
        tmps = P.alloc([8, 128], F32)
        P.memset("pool", hk[:], 0.0, writes=[hk])
        P.memset("pool", hv[:], 0.0, writes=[hv])
        hsel = C("hsel")
        P.tt("dve", SstF[:], Scf[:], coef[:, 0, 8, :].unsqueeze(2).to_broadcast([128, 8, 128]), ALU.mult, reads=[Scf, coef], writes=[SstF])
        P.tt("dve", SstB[:], Scb[:], coef[:, 1, 8, :].unsqueeze(2).to_broadcast([128, 8, 128]), ALU.mult, reads=[Scb, coef], writes=[SstB])
        for src in range(NCORE):
            xs = xs_p[src % 2]
            P.dma("sp", xs[:], xout.ap()[src * 128:(src + 1) * 128, :], reads=[xout], writes=[xs])
            xsk = xs[:, 0:512].rearrange("p (h w c) -> p h w c", h=2, w=2)
            xsv = xs[:, 512:1024].rearrange("p (w c) -> p w c", w=2)
            P.stt("dve", hk[:, 0], xsk[:, :, 1, :], hsel[:, src:src + 1], hk[:, 0], ALU.mult, ALU.add, reads=[xs, ck, hk], writes=[hk])
            P.stt("dve", hk[:, 1], xsk[:, :, 0, :], hsel[:, 8 + src:9 + src], hk[:, 1], ALU.mult, ALU.add, reads=[xs, ck, hk], writes=[hk])
            P.stt("dve", hv[:, 0, :], xsv[:, 1, :], hsel[:, src:src + 1], hv[:, 0, :], ALU.mult, ALU.add, reads=[xs, ck, hv], writes=[hv])
            P.stt("dve", hv[:, 1, :], xsv[:, 0, :], hsel[:, 8 + src:9 + src], hv[:, 1, :], ALU.mult, ALU.add, reads=[xs, ck, hv], writes=[hv])
            for dr, Sst in ((0, SstF), (1, SstB)):
                sv = xs[:, 1024 + dr * 1024:2048 + dr * 1024].rearrange("p (h e) -> p h e", h=8)
                P.tt("pool", tmps[:], sv, coef[:, dr, src, :].unsqueeze(2).to_broadcast([128, 8, 128]), ALU.mult, reads=[xs, coef], writes=[tmps])
                P.tt("dve", Sst[:], Sst[:], tmps[:], ALU.add, reads=[Sst, tmps], writes=[Sst])
        for h in range(2):
            P.cp("act", kTa[:, h, 256:384], hk[:, 0, h, :], reads=[hk], writes=[kTa])
            P.cp("act", kTa[:, h, 1408:1536], hk[:, 1, h, :], reads=[hk], writes=[kTa])
        P.cp("act", va[:, 2, :], hv[:, 0, :], reads=[hv], writes=[va])
        P.cp("act", va[:, 11, :], hv[:, 1, :], reads=[hv], writes=[va])
        P.pop()

        P.push()
        mk = P.alloc([NMK], BF16)
        P.dma("pool", mk[:], mk_d[:, :], writes=[mk])

        def M(name):
            a, b = MK[name]
            return mk[:, a:b]

        qc_p = [P.alloc([4, 128], BF16) for _ in range(2)]
        pt_p = [P.alloc([512], BF16) for _ in range(10)]
        den_p = [P.alloc([4, 128], F32) for _ in range(2)]
        ao_p = [P.alloc([4, 128], BF16) for _ in range(2)]
        it = 0
        ptc = 0
        for hk_ in range(2):
            for c in range(10):
                qc, den, ao = qc_p[it % 2], den_p[it % 2], ao_p[it % 2]
                it += 1
                P.dma("sp", qc[:], aq_d.ap()[4 * hk_:4 * hk_ + 4, :, c * 128:(c + 1) * 128].rearrange("h p t -> p h t"), reads=[(aq_d, range(4 * hk_, 4 * hk_ + 4))], writes=[qc])
                if c < 2:
                    blocks = [(0, None), (1, None)]
                else:
                    j = c - 2 + 1
                    blocks = [(0, None), (1, None), (2 + j - 1, "mL0" if c == 2 else "mL"), (2 + j, None), (2 + j + 1, "mR7" if c == 9 else "mR")]
                pts = []
                for (blk, mname) in blocks:
                    kcols = (blk * 128, blk * 128 + 128)
                    ps = pb()
                    P.mm(ps[:, :], kTa[:, hk_, kcols[0]:kcols[1]], qc[:], True, mname is None, reads=[kTa, qc], writes=[ps])
                    if mname is not None:
                        P.mm(ps[:, :], ident, M(mname), False, True, reads=[cb, mk], writes=[ps])
                    pt = pt_p[ptc % 10]
                    ptc += 1
                    P.act(pt[:], ps[:, :], AF.Exp, reads=[ps], writes=[pt], scale=128 ** -0.5)
                    pts.append((blk, pt))
                po, pd = pb(), pb()
                for i, (blk, pt) in enumerate(pts):
                    P.mm(po[:, :], va[:, blk, hk_ * 128:(hk_ + 1) * 128], pt[:], i == 0, i == len(pts) - 1, reads=[va, pt], writes=[po])
                for i, (blk, pt) in enumerate(pts):
                    P.mm(pd[:, :], ones, pt[:], i == 0, i == len(pts) - 1, reads=[cb, pt], writes=[pd])
                P.tt("dve", den[:], pd[:].rearrange("p (g q) -> p g q", g=4), sinkexp[:, 4 * hk_:4 * hk_ + 4].unsqueeze(2).to_broadcast([128, 4, 128]), ALU.add, reads=[pd, sinkexp], writes=[den])
                P.add("dve", lambda e, den=den: e.reciprocal(out=den[:], in_=den[:]), reads=[den], writes=[den])
                P.tt("dve", ao[:], po[:].rearrange("p (g q) -> p g q", g=4), den[:], ALU.mult, reads=[po, den], writes=[ao])
                P.dma("sp", br_d.ap()[4 * hk_:4 * hk_ + 4, :, c * 128:(c + 1) * 128].rearrange("h p t -> p h t"), ao[:], reads=[ao], writes=[(br_d, range(4 * hk_, 4 * hk_ + 4))])
        P.pop()

        P.push()
        SBc8 = [P.alloc([8, 128], BF16) for _ in range(8)]
        SBc = {c: SBc8[(c - 2) % 8] for c in range(10)}
        S32 = P.alloc([8, 128], F32)
        Sfb_p = [P.alloc([8, 128], BF16) for _ in range(2)]
        cnorm = PKc("c_norm")
        ld_p = [[P.alloc([8, 128], BF16) for _ in range(3)] for _ in range(2)]
        PT_p = [P.alloc([8, 128], BF16) for _ in range(2)]
        qf_p = [P.alloc([8, 128], BF16) for _ in range(1)] * 2
        qb_p = [P.alloc([8, 128], BF16) for _ in range(1)] * 2
        o32 = P.alloc([8, 128], F32)
        obf = P.alloc([8, 128], BF16)
        cen = P.alloc([8, 128], F32)
        sqr = P.alloc([8, 128], BF16)
        rsd = P.alloc([8, 128], F32)
        yo_p = [P.alloc([8, 128], BF16) for _ in range(1)] * 2
        oc = [0]

        def out_chunk(c, Sfb, Sbb, kt, vt):
            i = oc[0] % 2
            oc[0] += 1
            qc, kTc, gc = ld_p[i]
            PT, qf, qb, yo = PT_p[i], qf_p[i], qb_p[i], yo_p[i]
            cs = slice(c * 128, (c + 1) * 128)
            P.dma("sp", qc[:], rq_d.ap()[:, :, cs].rearrange("h p t -> p h t"), reads=[rq_d], writes=[qc])
            P.dma("sp", kTc[:], rk_d.ap()[:, :, cs].rearrange("h p t -> p h t"), reads=[rk_d], writes=[kTc])
            P.dma("sp", gc[:], rg_d.ap()[:, :, cs].rearrange("h p t -> p h t"), reads=[rg_d], writes=[gc])
            pa, pb2 = pb(), pb()
            for h in range(8):
                pp = pa if h < 4 else pb2
                P.mm(pp[:, (h % 4) * 128:(h % 4 + 1) * 128], kTc[:, h, :], qc[:, h, :], True, True, reads=[kTc, qc], writes=[pp])
            for half, pp in enumerate((pa, pb2)):
                P.tt("dve", PT[:, half * 4:half * 4 + 4, :], pp[:].rearrange("p (h i) -> p h i", h=4), MT[:, half * 4:half * 4 + 4, :], ALU.mult, reads=[pp, MT], writes=[PT])
            P.tt("pool", qf[:], qc[:], AFB[:, 0], ALU.mult, reads=[qc, AFB], writes=[qf])
            P.tt("pool", qb[:], qc[:], AFB[:, 1], ALU.mult, reads=[qc, AFB], writes=[qb])
            oa, ob = pb(), pb()
            for h in range(8):
                pp = oa if h < 4 else ob
                dst = pp[:, (h % 4) * 128:(h % 4 + 1) * 128]
                P.mm(dst, vt[:, h, :], PT[:, h, :], True, False, reads=[vt, PT], writes=[pp])
                P.mm(dst, Sfb[:, h, :], qf[:, h, :], False, False, reads=[Sfb, qf], writes=[pp])
                P.mm(dst, Sbb[:, h, :], qb[:, h, :], False, True, reads=[Sbb, qb], writes=[pp])
            for half, pp in enumerate((oa, ob)):
                P.act(o32[:, half * 4:half * 4 + 4, :], pp[:].rearrange("p (h i) -> p h i", h=4), AF.Copy, reads=[pp], writes=[o32])
            P.cp("pool", obf[:], o32[:], reads=[o32], writes=[obf])
            ma, mb = pb(), pb()
            for half, pp in enumerate((ma, mb)):
                P.mm(pp[:, :], ones, obf[:, half * 4:half * 4 + 4, :], True, True, reads=[cb, obf], writes=[pp])
                P.stt("dve", cen[:, half * 4:half * 4 + 4, :], pp[:].rearrange("p (h i) -> p h i", h=4), -1.0 / 128, o32[:, half * 4:half * 4 + 4, :], ALU.mult, ALU.add, reads=[pp, o32], writes=[cen])
            P.act(sqr[:], cen[:], AF.Square, reads=[cen], writes=[sqr])
            va_, vb_ = pb(), pb()
            for half, pp in enumerate((va_, vb_)):
                P.mm(pp[:, :], ones, sqr[:, half * 4:half * 4 + 4, :], True, True, reads=[cb, sqr], writes=[pp])
                P.act(rsd[:, half * 4:half * 4 + 4, :], pp[:].rearrange("p (h i) -> p h i", h=4), AF.Sqrt, reads=[pp, ck], writes=[rsd], bias=epsc, scale=1.0 / 128)
            P.add("dve", lambda e: e.reciprocal(out=rsd[:], in_=rsd[:]), reads=[rsd], writes=[rsd])
            P.tt("dve", cen[:], cen[:], rsd[:], ALU.mult, reads=[cen, rsd], writes=[cen])
            P.tt("pool", cen[:], cen[:], cnorm.unsqueeze(2).to_broadcast([128, 8, 128]), ALU.mult, reads=[cen, pkl], writes=[cen])
            P.tt("pool", yo[:], cen[:], gc[:], ALU.mult, reads=[cen, gc], writes=[yo])
            P.dma("sp", br_d.ap()[16:24, :, cs].rearrange("h p t -> p h t"), yo[:], reads=[yo], writes=[(br_d, range(16, 24))])

        def run_seq(chunks, startF, startB):
            if startB is None:
                P.memset("pool", S32[:], 0.0, writes=[S32])
            else:
                P.cp("pool", S32[:], startB[:], reads=[startB], writes=[S32])
            for c in reversed(chunks):
                P.cp("act", SBc[c][:], S32[:], reads=[S32], writes=[SBc[c]])
                if c != chunks[0]:
                    kt, vt = load_kv_chunk(c)
                    state_step_v(S32[:], S32, kt, vt, 1)
            if startF is None:
                P.memset("pool", S32[:], 0.0, writes=[S32])
            else:
                P.cp("pool", S32[:], startF[:], reads=[startF], writes=[S32])
            for n_, c in enumerate(chunks):
                Sfb = Sfb_p[n_ % 2]
                P.cp("act", Sfb[:], S32[:], reads=[S32], writes=[Sfb])
                kt, vt = load_kv_chunk(c)
                out_chunk(c, Sfb, SBc[c], kt, vt)
                if c != chunks[-1]:
                    state_step_v(S32[:], S32, kt, vt, 0)

        run_seq([0, 1], None, None)
        run_seq(list(range(2, 10)), SstF, SstB)
        P.pop()
        P.pop()

        wbr, wout = wfull["w_branch"][l % 2], wfull["w_out"][l % 2]
        P.push()
        brt = P.alloc([24, 512], BF16)
        merged = P.alloc([KC, 512], BF16)
        sg_p = [P.alloc([512], F32) for _ in range(3)]
        m_p = [P.alloc([512], F32) for _ in range(2)]
        t_p = [P.alloc([512], F32) for _ in range(2)]
        zt_p = [P.alloc([512], F32) for _ in range(3)]
        for g, (lo, hi) in enumerate(TG):
            n = hi - lo
            w = TGW[g]
            P.dma("sp", brt[:, :, 0:n], br_d.ap()[:, :, lo:hi].rearrange("k p t -> p k t"), reads=[br_d], writes=[brt])
            for j in range(KC):
                s1, s2, s3 = slot(), slot(), slot()
                gws = []
                for k in range(3):
                    sl_ = s1 if k < 2 else s2
                    v_ = sl_[:, (k % 2) * 2048:(k % 2) * 2048 + 2048].rearrange("p (k c) -> p k c", k=KC)
                    c0 = 7680 + k * 2048 + j * 128
                    load_w(v_, win.ap()[:, c0:c0 + 128].rearrange("(k p) c -> p k c", p=128), sl_, win)
                    gws.append((sl_, v_))
                wbv = s3[:, 0:3072].rearrange("p (k c) -> p k c", k=24)
                load_w(wbv, wbr.ap()[:, j * 128:(j + 1) * 128].rearrange("(k p) c -> p k c", p=128), s3, wbr)
                mt = m_p[j % 2]
                for k in range(3):
                    sl_, v_ = gws[k]
                    psg = pb()
                    for kc in range(KC):
                        P.mm(psg[:, 0:n], v_[:, kc, :], hT[:, kc, lo:hi], kc == 0, kc == KC - 1, reads=[sl_, (hT, kc)], writes=[psg])
                    sg = sg_p[k]
                    P.act(sg[:, 0:n], psg[:, 0:n], AF.Sigmoid, reads=[psg], writes=[sg])
                    psp = pb()
                    for kc in range(8):
                        P.mm(psp[:, 0:n], wbv[:, k * 8 + kc, :], brt[:, k * 8 + kc, 0:n], kc == 0, kc == 7, reads=[s3, brt], writes=[psp])
                    if k == 0:
                        P.tt("dve", mt[:, 0:n], sg[:, 0:n], psp[:, 0:n], ALU.mult, reads=[sg, psp], writes=[mt])
                    else:
                        tt_ = t_p[k - 1]
                        P.tt("dve", tt_[:, 0:n], sg[:, 0:n], psp[:, 0:n], ALU.mult, reads=[sg, psp], writes=[tt_])
                        if k == 1:
                            P.tt("pool", mt[:, 0:n], mt[:, 0:n], tt_[:, 0:n], ALU.add, reads=[mt, tt_], writes=[mt])
                        else:
                            P.tt("pool", merged[:, j, 0:n], mt[:, 0:n], tt_[:, 0:n], ALU.add, reads=[mt, tt_], writes=[merged])
            for jo in range(KC):
                sl = slot()
                wv = sl[:, 0:2048].rearrange("p (k c) -> p k c", k=KC)
                load_w(wv, wout.ap()[:, jo * 128:(jo + 1) * 128].rearrange("(k p) c -> p k c", p=128), sl, wout)
                ps = pb()
                for kc in range(KC):
                    P.mm(ps[:, 0:n], wv[:, kc, :], merged[:, kc, 0:n], kc == 0, kc == KC - 1, reads=[sl, merged], writes=[ps])
                zt = zt_p[jo % 3]
                P.dma("sp", zt[:, 0:n], z_d.ap()[jo, :, lo:hi], reads=[zs(jo, g)], writes=[zt])
                P.stt("dve", zt[:, 0:n], ps[:, 0:n], modT[:, 2 * 16 + jo, w:w + 1], zt[:, 0:n], ALU.mult, ALU.add, reads=[ps, modT, zt], writes=[zt])
                P.dma("sp", z_d.ap()[jo, :, lo:hi], zt[:, 0:n], reads=[zt], writes=[zs(jo, g)])
        P.pop()
        if debug and l == 0:
            dbg["z1"] = P.dram("dbg_z1", [KC, 128, T], F32, "ExternalOutput")
            dbg["br"] = P.dram("dbg_br", [24, 128, T], BF16, "ExternalOutput")
            dbg["z1_op"] = P.dma("sp", dbg["z1"].ap(), z_d.ap(), reads=[z_d], writes=[dbg["z1"]])
            dbg["br_op"] = P.dma("sp", dbg["br"].ap(), br_d.ap(), reads=[br_d], writes=[dbg["br"]])

        make_hT(1, 3)
        we1, we2 = wfull["w_e1"][l % 2], wfull["w_e2"][l % 2]
        P.push()
        wr32 = P.alloc([KC, 36], F32)
        wrh = P.alloc([KC, 36], BF16)
        wrl = P.alloc([KC, 36], BF16)
        P.dma("sp", wr32[:].rearrange("p k c -> p (k c)"), wr_d[l], writes=[wr32])
        P.cp("act", wrh[:], wr32[:], reads=[wr32], writes=[wrh])
        P.tt("dve", wrl[:], wr32[:], wrh[:], ALU.subtract, reads=[wr32, wrh], writes=[wrl])
        selT = P.alloc([4096], BF16)
        P.dma("pool", selT[0:32, :], sel_d[:, :], writes=[selT])
        WT = P.alloc([T], BF16)
        rbias = PKc("r_bias")
        lg_p = [P.alloc([36], F32) for _ in range(2)]
        sm_p = [P.alloc([64], F32) for _ in range(2)]
        el_p = [P.alloc([4, 8], F32) for _ in range(2)]
        e2_p = [P.alloc([4, 8], F32) for _ in range(2)]
        oh_p = [P.alloc([2, 32], F32) for _ in range(2)]
        W_p = [P.alloc([32], F32) for _ in range(2)]
        Wb_p = [P.alloc([32], BF16) for _ in range(2)]
        for c in range(10):
            lg, sm, el, e2, oh, W_, Wb_ = lg_p[c % 2], sm_p[c % 2], el_p[c % 2], e2_p[c % 2], oh_p[c % 2], W_p[c % 2], Wb_p[c % 2]
            ps = pb()
            for i, wpart in enumerate((wrh, wrl)):
                for kc in range(KC):
                    P.mm(ps[:, 0:36], hT[:, kc, c * 128:(c + 1) * 128], wpart[:, kc, :], i == 0 and kc == 0, i == 1 and kc == KC - 1, reads=[(hT, kc), wpart], writes=[ps])
            P.tt("dve", lg[:], ps[:, 0:36], rbias, ALU.add, reads=[ps, pkl], writes=[lg])
            P.add("dve", lambda e, sm=sm, lg=lg: e.tensor_reduce(out=sm[:, 0:1], in_=lg[:, 0:4], axis=AX.X, op=ALU.max), reads=[lg], writes=[sm])
            P.ts("dve", sm[:, 1:2], sm[:, 0:1], -1.0, None, ALU.mult, None, reads=[sm], writes=[sm])
            P.act(sm[:, 16:20], lg[:, 0:4], AF.Exp, reads=[lg, sm], writes=[sm], bias=sm[:, 1:2], scale=1.0)
            P.add("dve", lambda e, sm=sm: e.tensor_reduce(out=sm[:, 2:3], in_=sm[:, 16:20], axis=AX.X, op=ALU.add), reads=[sm], writes=[sm])
            P.add("dve", lambda e, sm=sm: e.reciprocal(out=sm[:, 3:4], in_=sm[:, 2:3]), reads=[sm], writes=[sm])
            P.ts("dve", sm[:, 20:24], lg[:, 0:4], sm[:, 0:1], None, ALU.is_equal, None, reads=[lg, sm], writes=[sm])
            P.ts("dve", sm[:, 24:28], sm[:, 20:24], -1.0, 1e30, ALU.add, ALU.mult, reads=[sm], writes=[sm])
            P.tt("dve", el[:], lg[:, 4:36].rearrange("p (g e) -> p g e", g=4), sm[:, 24:28].unsqueeze(2).to_broadcast([128, 4, 8]), ALU.add, reads=[lg, sm], writes=[el])
            elf = el[:].rearrange("p g e -> p (g e)")
            e2f = e2[:].rearrange("p g e -> p (g e)")
            P.add("dve", lambda e, sm=sm, elf=elf: e.tensor_reduce(out=sm[:, 4:5], in_=elf, axis=AX.X, op=ALU.max), reads=[el], writes=[sm])
            P.ts("dve", oh[:, 0, :], elf, sm[:, 4:5], None, ALU.is_equal, None, reads=[el, sm], writes=[oh])
            P.stt("dve", e2f, oh[:, 0, :], -1e30, elf, ALU.mult, ALU.add, reads=[oh, el], writes=[e2])
            P.add("dve", lambda e, sm=sm, e2f=e2f: e.tensor_reduce(out=sm[:, 5:6], in_=e2f, axis=AX.X, op=ALU.max), reads=[e2], writes=[sm])
            P.ts("dve", oh[:, 1, :], e2f, sm[:, 5:6], None, ALU.is_equal, None, reads=[e2, sm], writes=[oh])
            P.tt("dve", sm[:, 6:7], sm[:, 5:6], sm[:, 4:5], ALU.subtract, reads=[sm], writes=[sm])
            P.act(sm[:, 7:8], sm[:, 6:7], AF.Exp, reads=[sm], writes=[sm])
            P.ts("dve", sm[:, 8:9], sm[:, 7:8], 1.0, None, ALU.add, None, reads=[sm], writes=[sm])
            P.add("dve", lambda e, sm=sm: e.reciprocal(out=sm[:, 8:9], in_=sm[:, 8:9]), reads=[sm], writes=[sm])
            P.tt("dve", sm[:, 9:10], sm[:, 8:9], sm[:, 3:4], ALU.mult, reads=[sm], writes=[sm])
            P.tt("dve", sm[:, 10:11], sm[:, 9:10], sm[:, 7:8], ALU.mult, reads=[sm], writes=[sm])
            P.ts("dve", W_[:], oh[:, 0, :], sm[:, 9:10], None, ALU.mult, None, reads=[oh, sm], writes=[W_])
            P.stt("dve", W_[:], oh[:, 1, :], sm[:, 10:11], W_[:], ALU.mult, ALU.add, reads=[oh, sm, W_], writes=[W_])
            P.cp("act", Wb_[:], W_[:], reads=[W_], writes=[Wb_])
            P.tr(pbt[0:32, 0:128], Wb_[:], ident, reads=[Wb_, cb], writes=[pbt])
            P.cp("act", WT[0:32, c * 128:(c + 1) * 128], pbt[0:32, 0:128], reads=[pbt], writes=[WT])
        if debug and l == 0:
            dbg["WT"] = P.dram("dbg_WT", [32, T], BF16, "ExternalOutput")
            dbg["WT_op"] = P.dma("sp", dbg["WT"].ap(), WT[0:32, :], reads=[WT], writes=[dbg["WT"]])

        acc = P.alloc([KC, 512], F32)
        wbt_p = [P.alloc([512], BF16) for _ in range(2)]
        sgl_p = [P.alloc([512], BF16) for _ in range(2)]
        a_p = [[P.alloc([512], BF16) for _ in range(4)] for _ in range(2)]
        zt2_p = [P.alloc([512], F32) for _ in range(3)]
        for g, (lo, hi) in enumerate(TG):
            n = hi - lo
            w = TGW[g]
            P.memset("dve", acc[:], 0.0, writes=[acc])
            for e_ in range(n_experts):
                wbt = wbt_p[e_ % 2]
                ps = pb()
                P.mm(ps[:, 0:n], selT[0:32, e_ * 128:(e_ + 1) * 128], WT[0:32, lo:hi], True, True, reads=[selT, WT], writes=[ps])
                P.act(wbt[:, 0:n], ps[:, 0:n], AF.Copy, reads=[ps], writes=[wbt])
                aj = a_p[e_ % 2]
                for j in range(4):
                    sl = slot()
                    slv = sl[:].rearrange("p (k c) -> p k c", k=KC)
                    r0 = e_ * 2048
                    load_w(slv[:, :, 0:128], we1.ap()[r0:r0 + 2048, j * 128:(j + 1) * 128].rearrange("(k p) c -> p k c", p=128), sl, we1)
                    load_w(slv[:, :, 128:256], we1.ap()[r0:r0 + 2048, 512 + j * 128:512 + (j + 1) * 128].rearrange("(k p) c -> p k c", p=128), sl, we1)
                    psg, psu = pb(), pb()
                    for kc in range(KC):
                        P.mm(psg[:, 0:n], slv[:, kc, 0:128], hT[:, kc, lo:hi], kc == 0, kc == KC - 1, reads=[sl, (hT, kc)], writes=[psg])
                    for kc in range(KC):
                        P.mm(psu[:, 0:n], slv[:, kc, 128:256], hT[:, kc, lo:hi], kc == 0, kc == KC - 1, reads=[sl, (hT, kc)], writes=[psu])
                    sgl = sgl_p[j % 2]
                    P.act(sgl[:, 0:n], psg[:, 0:n], AF.Silu, reads=[psg], writes=[sgl])
                    P.tt("dve", aj[j][:, 0:n], sgl[:, 0:n], psu[:, 0:n], ALU.mult, reads=[sgl, psu], writes=[aj[j]])
                    P.tt("dve", aj[j][:, 0:n], aj[j][:, 0:n], wbt[:, 0:n], ALU.mult, reads=[aj[j], wbt], writes=[aj[j]])
                w2s = []
                for half in range(2):
                    sl = slot()
                    v_ = sl[:].rearrange("p (j c) -> p j c", j=2)
                    r0 = e_ * 512 + half * 256
                    load_w(v_, we2.ap()[r0:r0 + 256, :].rearrange("(j p) c -> p j c", p=128), sl, we2)
                    w2s.append((sl, v_))
                for jo in range(KC):
                    ps = pb()
                    for j in range(4):
                        sl, v_ = w2s[j // 2]
                        P.mm(ps[:, 0:n], v_[:, j % 2, jo * 128:(jo + 1) * 128], aj[j][:, 0:n], j == 0, j == 3, reads=[sl, aj[j]], writes=[ps])
                    P.tt("dve", acc[:, jo, 0:n], acc[:, jo, 0:n], ps[:, 0:n], ALU.add, reads=[acc, ps], writes=[acc])
            for jo in range(KC):
                zt = zt2_p[jo % 3]
                P.dma("sp", zt[:, 0:n], z_d.ap()[jo, :, lo:hi], reads=[zs(jo, g)], writes=[zt])
                P.stt("dve", zt[:, 0:n], acc[:, jo, 0:n], modT[:, 5 * 16 + jo, w:w + 1], zt[:, 0:n], ALU.mult, ALU.add, reads=[acc, modT, zt], writes=[zt])
                P.dma("sp", z_d.ap()[jo, :, lo:hi], zt[:, 0:n], reads=[zt], writes=[zs(jo, g)])
        P.pop()

    fin = []
    P.push()
    ob_p = [P.alloc([LAT], F32) for _ in range(2)]
    for kc in range(KC):
        ob = ob_p[kc % 2]
        P.dma("sp", ob[:], z_d.ap()[kc, :, CTX:T], reads=[zs(kc)], writes=[ob])
        fin.append(P.dma("sp", out_d[kc], ob[:], reads=[ob], writes=[out_d]))
    P.pop()
    for k_, v_ in dbg.items():
        if k_.endswith("_op"):
            fin.append(v_)
    print("ops:", {e: len(P.q[e]) for e in P.ENGS}, "sbuf peak cols:", P.peak, "of", P.AW)
    P.emit(final_wait_ops=fin)
    P.close()
    return nc


def _consts(core):
    j = np.arange(128)
    ck = np.zeros((128, NCK), np.float32)
    a, b = CK["cj"]
    ck[:, a] = 127 - j
    ck[:, a + 1] = j
    n = np.zeros((2, 9), np.float32)
    v = np.zeros((2, 9), np.float32)
    for s in range(8):
        if s < core:
            n[0, s] = 1024.0 * (core - 1 - s)
            v[0, s] = 1.0
        if s > core:
            n[1, s] = 1024.0 * (s - core - 1)
            v[1, s] = 1.0
    n[0, 8] = 1024.0 * core
    v[0, 8] = 1.0
    n[1, 8] = 1024.0 * (7 - core)
    v[1, 8] = 1.0
    a, b = CK["coefn"]
    ck[:, a:b] = n.reshape(1, 18)
    a, b = CK["coefv"]
    ck[:, a:b] = v.reshape(1, 18)
    hs = np.zeros(16, np.float32)
    if core > 0:
        hs[core - 1] = 1.0
    if core < 7:
        hs[8 + core + 1] = 1.0
    a, b = CK["hsel"]
    ck[:, a:b] = hs[None, :]
    a, b = CK["eps"]
    ck[:, a] = EPS
    cb = np.zeros((128, NCB), np.float32)
    a, b = CB["ident"]
    cb[:, a:b] = np.eye(128)
    a, b = CB["ones"]
    cb[:, a:b] = 1.0
    pm = np.zeros((128, 128), np.float32)
    for d in range(128):
        if d % 64 < 32:
            pm[d + 32, d] = -1.0
        else:
            pm[d - 32, d] = 1.0
    a, b = CB["perm"]
    cb[:, a:b] = pm
    rc = np.zeros((128, NRC), np.float32)
    jj, ii = np.meshgrid(j, j, indexing="ij")
    for name, val in (("D1", np.maximum(ii - jj, 0)), ("U1", (ii >= jj)), ("D2", np.maximum(jj - ii, 0)), ("U2", (jj >= ii)),
                      ("I1", ii + 1), ("I2", 128 - ii)):
        a, b = RC[name]
        rc[:, a:b] = val
    mk = np.zeros((128, NMK), np.float32)
    kk, qq = np.meshgrid(j, j, indexing="ij")
    mL = np.where(qq <= kk, 0.0, NEG)
    mR = np.where(kk <= qq, 0.0, NEG)
    full = np.full((128, 128), NEG)
    for name, m in (("mL0", full if core == 0 else mL), ("mL", mL), ("mR", mR), ("mR7", full if core == 7 else mR)):
        a, b = MK[name]
        mk[:, a:b] = np.tile(m, (1, 4))
    t = core * LAT + np.arange(LAT)
    row, col = (t // 64).astype(np.float32), (t % 64).astype(np.float32)
    inv = (10000.0 ** (-np.arange(32, dtype=np.float32) / 32)).astype(np.float32)
    d = np.arange(128)
    pos = np.where((d < 64)[:, None], row[None, :], col[None, :]).astype(np.float32)
    ang = pos * inv[d % 32][:, None]
    rope = np.concatenate([np.cos(ang), np.sin(ang)], axis=1).astype(np.float32)
    sel = np.zeros((32, 4096), np.float32)
    for e in range(32):
        sel[e, e * 128:(e + 1) * 128] = 1.0
    return ck, cb, rc, mk, rope, sel


def _fm(vec):
    return np.ascontiguousarray(vec.reshape(16, 128).T)


def kernel(x, c, ctx, c_ctx, norm_mix, norm_ffn, w_ada, b_ada, w_in, a_q_norm, a_k_norm, a_sink, b_norm, b_spatial,
           b_spatial_bias, c_decay_fwd, c_decay_bwd, c_norm, w_branch, w_out, w_router_group, b_router_group,
           w_router_expert, b_router_expert, w_expert_in, w_expert_out, _depth=DEPTH, _debug=False, _n_experts=32):
    f = lambda a: np.asarray(a, np.float32)
    x, c, ctx, c_ctx = f(x), f(c), f(ctx), f(c_ctx)
    depth = _depth
    nc = build_program(depth, _n_experts, _debug)
    pk = np.zeros((depth, 128, NPK), np.float32)
    bsp = np.zeros((depth, 128, 1024), np.float32)
    pk2 = np.zeros((depth, 128, 2048), np.float32)
    wr = np.zeros((depth, 128, KC * 36), np.float32)
    for l in range(depth):
        def put(name, val):
            a, b = PK[name]
            pk[l, :, a:b] = val
        put("norm_mix", _fm(f(norm_mix)[l]))
        put("norm_ffn", _fm(f(norm_ffn)[l]))
        put("b_ada", f(b_ada)[l].reshape(96, 128).T)
        put("a_q_norm", f(a_q_norm)[l][:, None])
        put("a_k_norm", f(a_k_norm)[l][:, None])
        put("c_norm", f(c_norm)[l].T)
        put("a_sink", f(a_sink)[l][None, :])
        put("c_decay", np.concatenate([f(c_decay_fwd)[l], f(c_decay_bwd)[l]])[None, :])
        put("r_bias", np.concatenate([f(b_router_group)[l], f(b_router_expert)[l]])[None, :])
        pk2[l, :, 0:1024] = f(b_spatial_bias)[l].reshape(1, 1024)
        pk2[l, :, 1024:2048] = f(b_norm)[l].reshape(1, 1024)
        bsp[l] = f(b_spatial)[l].transpose(2, 0, 1).reshape(128, 1024)
        wrl = np.concatenate([f(w_router_group)[l], f(w_router_expert)[l]], axis=1)
        wr[l] = wrl.reshape(16, 128, 36).transpose(1, 0, 2).reshape(128, KC * 36)
    wfull = {"w_ada": f(w_ada)[:depth], "w_in": f(w_in)[:depth], "w_branch": f(w_branch)[:depth].reshape(depth, 3072, 2048),
             "w_out": f(w_out)[:depth], "w_e1": f(w_expert_in)[:depth].reshape(depth, 32 * 2048, 1024),
             "w_e2": f(w_expert_out)[:depth].reshape(depth, 32 * 512, 2048)}
    in_maps = []
    silu_in = np.stack([_fm(c[0]), _fm(c_ctx)], axis=2).reshape(128, 32)
    for core in range(NCORE):
        ck, cb, rc, mk, rope, sel = _consts(core)
        a, b = CK["silu_in"]
        ck[:, a:b] = silu_in
        tok = np.concatenate([ctx[0], x[0, core * LAT:(core + 1) * LAT]], axis=0)
        z0 = np.ascontiguousarray(tok.T.reshape(16, 128, T))
        m = {"z0": z0, "pk": pk, "ck": ck, "cb": cb, "rc": rc, "mk": mk, "rope": rope, "sel": sel, "bsp": bsp, "wr": wr, "pk2": pk2}
        for n, (r, cc_) in WSPEC.items():
            rs = r // NCORE
            m[n + "_s"] = np.ascontiguousarray(wfull[n][:, core * rs:(core + 1) * rs, :])
        in_maps.append(m)
    res = run_bass_kernel_spmd(nc, in_maps, core_ids=list(range(NCORE)))
    outs = []
    for core in range(NCORE):
        o = res.results[core]["out"]
        outs.append(o.reshape(D, LAT).T)
    out = np.concatenate(outs, axis=0)[None].astype(np.float32)
    if _debug:
        return out, res.results
    return out
```
